# Optimizing a Trainium2 kernel written in Bass

```python
import jax, jax.numpy as jnp
from jax import lax
import numpy as np

D_MODEL = 1024
BATCH = 2
SEQ = 8192
DEPTH = 2

PLE_DIM = 256
EPS = 1e-6
CHUNK = 64
ROPE_THETA = 10000.0
RET_HEADS = 4
RET_DK = 128
RET_DV = 256
RET_QK = RET_HEADS * RET_DK
RET_V = RET_HEADS * RET_DV
GDN_HEADS = 8
GDN_DK = 128
GDN_DV = 128
GDN_QK = GDN_HEADS * GDN_DK
GDN_V = GDN_HEADS * GDN_DV
GDN_CONV_DIM = 2 * GDN_QK + GDN_V
CONV_WIDTH = 4
D_FF = 2816
N_EXPERTS = 8
TOP_K = 2
D_FF_EXPERT = 1408
N_DENSE = (DEPTH + 1) // 2
N_MOE = DEPTH // 2
OFF_RQ = RET_QK
OFF_RK = OFF_RQ + RET_QK
OFF_RV = OFF_RK + RET_V
OFF_RG = OFF_RV + RET_V
OFF_GQKV = OFF_RG + GDN_CONV_DIM
OFF_GZ = OFF_GQKV + GDN_V
OFF_GA = OFF_GZ + GDN_HEADS
OFF_GB = OFF_GA + GDN_HEADS
OFF_MG_RET = OFF_GB + D_MODEL
D_IN = OFF_MG_RET + D_MODEL
SPLIT_IDX = (OFF_RQ, OFF_RK, OFF_RV, OFF_RG, OFF_GQKV, OFF_GZ, OFF_GA, OFF_GB, OFF_MG_RET)

kernel_name = "hybrid_retention_gdn_moe_ple"


def rmsnorm(x, g):
    x32 = x.astype(jnp.float32)
    y = x32 * lax.rsqrt(jnp.mean(x32 * x32, axis=-1, keepdims=True) + EPS)
    return (y * g.astype(jnp.float32)).astype(x.dtype)


def l2norm(x):
    return x * lax.rsqrt(jnp.sum(x * x, axis=-1, keepdims=True) + EPS)


def rotary(x, pos):
    half = x.shape[-1] // 2
    inv_freq = ROPE_THETA ** (-jnp.arange(half, dtype=jnp.float32) / half)
    ang = pos[:, None] * inv_freq[None, :]
    cos = jnp.cos(ang)[None, :, None, :]
    sin = jnp.sin(ang)[None, :, None, :]
    x1, x2 = x[..., :half], x[..., half:]
    return jnp.concatenate([x1 * cos - x2 * sin, x2 * cos + x1 * sin], axis=-1)


def causal_conv(x, w):
    width, ch = w.shape
    return lax.conv_general_dilated(
        x, w[:, None, :].astype(x.dtype), window_strides=(1,), padding=[(width - 1, 0)],
        dimension_numbers=("NWC", "WIO", "NWC"), feature_group_count=ch)


def retention(q, k, v):
    B, T, H, dk = q.shape
    dv = v.shape[-1]
    n = T // CHUNK
    log_gamma = jnp.log1p(-jnp.exp2(-5.0 - jnp.arange(H, dtype=jnp.float32)))
    k = k * (dk ** -0.5)
    qc = q.reshape(B, n, CHUNK, H, dk)
    kc = k.reshape(B, n, CHUNK, H, dk)
    vc = v.reshape(B, n, CHUNK, H, dv)
    idx = jnp.arange(CHUNK, dtype=jnp.float32)
    rel = idx[:, None] - idx[None, :]
    causal = rel >= 0
    dmask = jnp.where(causal[None], jnp.exp(jnp.where(causal, rel, 0.0)[None] * log_gamma[:, None, None]), 0.0)
    scores = jnp.einsum("bnihd,bnjhd->bnhij", qc, kc) * dmask
    intra = jnp.einsum("bnhij,bnjhv->bnihv", scores, vc)
    q_dec = qc * jnp.exp((idx + 1.0)[:, None] * log_gamma[None, :])[:, :, None]
    k_dec = kc * jnp.exp((CHUNK - 1.0 - idx)[:, None] * log_gamma[None, :])[:, :, None]
    chunk_decay = jnp.exp(CHUNK * log_gamma)[None, :, None, None]

    def step(R, xs):
        qd, kd, vv = xs
        inter = jnp.einsum("bchd,bhdv->bchv", qd, R)
        R = R * chunk_decay + jnp.einsum("bchd,bchv->bhdv", kd, vv)
        return R, inter

    R0 = jnp.zeros((B, H, dk, dv), jnp.float32)
    _, inter = lax.scan(step, R0, (q_dec.swapaxes(0, 1), k_dec.swapaxes(0, 1), vc.swapaxes(0, 1)))
    out = intra + inter.swapaxes(0, 1)
    return out.reshape(B, T, H, dv)


def gated_delta_rule(q, k, v, g, beta):
    B, T, H, dk = q.shape
    dv = v.shape[-1]
    n = T // CHUNK
    q = q * (dk ** -0.5)

    def to_chunks(t):
        return t.reshape(B, n, CHUNK, H, -1).transpose(0, 3, 1, 2, 4)

    qc, kc, vc = to_chunks(q), to_chunks(k), to_chunks(v)
    gc = jnp.cumsum(g.reshape(B, n, CHUNK, H).transpose(0, 3, 1, 2), axis=-1)
    bc = beta.reshape(B, n, CHUNK, H).transpose(0, 3, 1, 2)[..., None]
    incl = jnp.tril(jnp.ones((CHUNK, CHUNK), bool))
    strict = jnp.tril(jnp.ones((CHUNK, CHUNK), bool), -1)
    diff = gc[..., :, None] - gc[..., None, :]
    decay_incl = jnp.where(incl, jnp.exp(jnp.where(incl, diff, 0.0)), 0.0)
    kb = kc * bc
    a_mat = jnp.einsum("bhnid,bhnjd->bhnij", kb, kc) * jnp.where(strict, decay_incl, 0.0)
    lhs = jnp.eye(CHUNK, dtype=jnp.float32) + a_mat
    u = lax.linalg.triangular_solve(lhs, vc * bc, left_side=True, lower=True, unit_diagonal=True)
    w = lax.linalg.triangular_solve(lhs, kb * jnp.exp(gc)[..., None], left_side=True, lower=True, unit_diagonal=True)
    qk = jnp.einsum("bhnid,bhnjd->bhnij", qc, kc) * decay_incl
    q_dec = qc * jnp.exp(gc)[..., None]
    g_last = gc[..., -1]
    k_dec = kc * jnp.exp(g_last[..., None] - gc)[..., None]

    def step(S, xs):
        qd, qkn, kd, un, wn, gl = xs
        v_new = un - jnp.einsum("bhcd,bhdv->bhcv", wn, S)
        o = jnp.einsum("bhcd,bhdv->bhcv", qd, S) + jnp.einsum("bhij,bhjv->bhiv", qkn, v_new)
        S = S * jnp.exp(gl)[..., None, None] + jnp.einsum("bhcd,bhcv->bhdv", kd, v_new)
        return S, o

    xs = tuple(jnp.moveaxis(t, 2, 0) for t in (q_dec, qk, k_dec, u, w, g_last))
    S0 = jnp.zeros((B, H, dk, dv), jnp.float32)
    _, o = lax.scan(step, S0, xs)
    return jnp.moveaxis(o, 0, 2).transpose(0, 2, 3, 1, 4).reshape(B, T, H, dv)


def mixer_block(h, g_norm, w_in, conv_w, a_log, dt_bias, ret_gn, gdn_gn, w_br_ret, w_br_gdn, w_out, pos):
    B, T, _ = h.shape
    f32 = jnp.float32
    u = rmsnorm(h, g_norm)
    proj = u @ w_in
    rq, rk, rv, rg, gqkv, gz, ga, gb, mg_ret, mg_gdn = jnp.split(proj, SPLIT_IDX, axis=-1)
    q = rotary(rq.reshape(B, T, RET_HEADS, RET_DK).astype(f32), pos)
    k = rotary(rk.reshape(B, T, RET_HEADS, RET_DK).astype(f32), pos)
    v = rv.reshape(B, T, RET_HEADS, RET_DV).astype(f32)
    o = retention(q, k, v)
    mu = jnp.mean(o, axis=-1, keepdims=True)
    var = jnp.mean(jnp.square(o - mu), axis=-1, keepdims=True)
    o = (o - mu) * lax.rsqrt(var + EPS) * ret_gn.astype(f32).reshape(RET_HEADS, RET_DV)
    y_ret = (o.reshape(B, T, RET_V).astype(h.dtype) * jax.nn.silu(rg)) @ w_br_ret
    qkv = jax.nn.silu(causal_conv(gqkv, conv_w))
    gq, gk, gv = jnp.split(qkv, (GDN_QK, 2 * GDN_QK), axis=-1)
    q = l2norm(gq.reshape(B, T, GDN_HEADS, GDN_DK).astype(f32))
    k = l2norm(gk.reshape(B, T, GDN_HEADS, GDN_DK).astype(f32))
    v = gv.reshape(B, T, GDN_HEADS, GDN_DV).astype(f32)
    beta = jax.nn.sigmoid(gb.astype(f32))
    g = -jnp.exp(a_log.astype(f32)) * jax.nn.softplus(ga.astype(f32) + dt_bias.astype(f32))
    o = gated_delta_rule(q, k, v, g, beta)
    o = o * lax.rsqrt(jnp.mean(o * o, axis=-1, keepdims=True) + EPS) * gdn_gn.astype(f32)
    y_gdn = (o.reshape(B, T, GDN_V).astype(h.dtype) * jax.nn.silu(gz)) @ w_br_gdn
    merged = jax.nn.sigmoid(mg_ret) * y_ret + jax.nn.sigmoid(mg_gdn) * y_gdn
    return h + merged @ w_out


def swiglu(x, wg, wu, wd):
    return (jax.nn.silu(x @ wg) * (x @ wu)) @ wd


def moe(x, router, wg, wu, wd):
    logits = (x @ router).astype(jnp.float32)
    top_logit, top_idx = lax.top_k(logits, TOP_K)
    top_w = jax.nn.softmax(top_logit, axis=-1)
    gates = jnp.sum(jax.nn.one_hot(top_idx, N_EXPERTS, dtype=jnp.float32) * top_w[..., None], axis=-2)
    y = jnp.zeros_like(x)
    for e in range(N_EXPERTS):
        y = y + gates[..., e:e + 1].astype(x.dtype) * swiglu(x, wg[e], wu[e], wd[e])
    return y


def setup_inputs(seed: int = 0) -> dict:
    key = jax.random.key(seed)
    ks = jax.random.split(key, 26)
    f32 = jnp.float32

    def nrm(k, shape, fan_in):
        return jax.random.normal(k, shape, f32) * (fan_in ** -0.5)

    def gain(k, shape):
        return 1.0 + 0.02 * jax.random.normal(k, shape, f32)

    dt = jnp.exp(jax.random.uniform(ks[5], (DEPTH, GDN_HEADS), f32, np.log(1e-3), np.log(1e-1)))
    return {
        "x": jax.random.normal(ks[0], (BATCH, SEQ, D_MODEL), f32),
        "p": jax.random.normal(ks[1], (DEPTH, BATCH, SEQ, PLE_DIM), f32),
        "w_in": nrm(ks[2], (DEPTH, D_MODEL, D_IN), D_MODEL),
        "conv_w": 0.5 * jax.random.normal(ks[3], (DEPTH, CONV_WIDTH, GDN_CONV_DIM), f32),
        "a_log": jnp.log(jax.random.uniform(ks[4], (DEPTH, GDN_HEADS), f32, 1.0, 16.0)),
        "dt_bias": dt + jnp.log(-jnp.expm1(-dt)),
        "ret_gn": gain(ks[6], (DEPTH, RET_V)),
        "gdn_gn": gain(ks[7], (DEPTH, GDN_DV)),
        "w_br_ret": nrm(ks[8], (DEPTH, RET_V, D_MODEL), RET_V),
        "w_br_gdn": nrm(ks[9], (DEPTH, GDN_V, D_MODEL), GDN_V),
        "w_out": nrm(ks[10], (DEPTH, D_MODEL, D_MODEL), D_MODEL),
        "norm_mix": gain(ks[11], (DEPTH, D_MODEL)),
        "norm_ffn": gain(ks[12], (DEPTH, D_MODEL)),
        "norm_ple": gain(ks[13], (DEPTH, D_MODEL)),
        "w_ple": nrm(ks[14], (DEPTH, PLE_DIM, D_MODEL), PLE_DIM),
        "w_ple_gate": nrm(ks[15], (DEPTH, D_MODEL, D_MODEL), D_MODEL),
        "ffn_w_gate": nrm(ks[16], (N_DENSE, D_MODEL, D_FF), D_MODEL),
        "ffn_w_up": nrm(ks[17], (N_DENSE, D_MODEL, D_FF), D_MODEL),
        "ffn_w_down": nrm(ks[18], (N_DENSE, D_FF, D_MODEL), D_FF),
        "router": nrm(ks[19], (N_MOE, D_MODEL, N_EXPERTS), D_MODEL),
        "exp_w_gate": nrm(ks[20], (N_MOE, N_EXPERTS, D_MODEL, D_FF_EXPERT), D_MODEL),
        "exp_w_up": nrm(ks[21], (N_MOE, N_EXPERTS, D_MODEL, D_FF_EXPERT), D_MODEL),
        "exp_w_down": nrm(ks[22], (N_MOE, N_EXPERTS, D_FF_EXPERT, D_MODEL), D_FF_EXPERT),
        "norm_final": gain(ks[23], (D_MODEL,)),
    }


def reference(x, p, w_in, conv_w, a_log, dt_bias, ret_gn, gdn_gn, w_br_ret, w_br_gdn, w_out,
              norm_mix, norm_ffn, norm_ple, w_ple, w_ple_gate, ffn_w_gate, ffn_w_up, ffn_w_down,
              router, exp_w_gate, exp_w_up, exp_w_down, norm_final):
    pos = jnp.arange(x.shape[1], dtype=jnp.float32)
    h = x
    for i in range(DEPTH):
        h = mixer_block(h, norm_mix[i], w_in[i], conv_w[i], a_log[i], dt_bias[i], ret_gn[i], gdn_gn[i],
                        w_br_ret[i], w_br_gdn[i], w_out[i], pos)
        u = rmsnorm(h, norm_ffn[i])
        j = i // 2
        if i % 2 == 0:
            h = h + swiglu(u, ffn_w_gate[j], ffn_w_up[j], ffn_w_down[j])
        else:
            h = h + moe(u, router[j], exp_w_gate[j], exp_w_up[j], exp_w_down[j])
        ple = p[i] @ w_ple[i]
        h = h + jax.nn.sigmoid(rmsnorm(h, norm_ple[i]) @ w_ple_gate[i]) * ple
    return rmsnorm(h, norm_final)
```

```python
import math
import numpy as np
import ml_dtypes
import concourse.bass as bass
import concourse.mybir as mybir
from concourse.bass_utils import run_bass_kernel_spmd

F32 = mybir.dt.float32
BF16 = mybir.dt.bfloat16
I32 = mybir.dt.int32
AF = mybir.ActivationFunctionType
ALU = mybir.AluOpType

D = 1024
T = 8192
NB = 2
NCORE = 8
TOK = 2048
EPS = 1e-6
DFF = 2816
NEXP = 8
DFE = 1408
PLE = 256


class Prog:
    ENG = ("pe", "act", "dve", "pool", "sp")

    def __init__(self, nc, self_wait=True):
        self.nc = nc
        self.q = {e: [] for e in self.ENG}
        self.semh = {}
        self.ecnt = {}
        for e in ("pe", "act", "dve", "pool"):
            self.semh["E_" + e] = nc.alloc_semaphore("E_" + e)
            self.ecnt[e] = 0
        self.seen = {e: {} for e in self.ENG}
        self.res = {}
        self.dcnt = {}
        self.self_wait = self_wait
        self.uid = 0

    def _deps(self, eng, r, w):
        deps = {}

        def add(s, v):
            if deps.get(s, 0) < v:
                deps[s] = v
        for k in r:
            st = self.res.get(k)
            if st and st[0]:
                add(*st[0])
        for k in w:
            st = self.res.get(k)
            if st:
                if st[0]:
                    add(*st[0])
                for s, v in st[1].items():
                    add(s, v)
        waits = []
        own = "E_" + eng
        for s, v in deps.items():
            if self.seen[eng].get(s, 0) >= v:
                continue
            if s == own and (eng == "pe" or not self.self_wait):
                continue
            waits.append((s, v))
            self.seen[eng][s] = v
        return waits

    def _commit(self, tok, r, w):
        for k in w:
            self.res[k] = [tok, {}]
        for k in r:
            st = self.res.setdefault(k, [None, {}])
            st[1][tok[0]] = tok[1]

    def op(self, eng, fn, r=(), w=()):
        waits = self._deps(eng, r, w)
        self.ecnt[eng] += 1
        tok = ("E_" + eng, self.ecnt[eng])
        self.q[eng].append((waits, fn, (tok[0], 1)))
        self._commit(tok, r, w)
        return tok

    def dma(self, qeng, out, in_, r=(), w=(), slot=None):
        if slot is None:
            slot = (tuple(w) + tuple(r))[0]
        sname = "D_" + str(slot)
        if sname not in self.semh:
            self.semh[sname] = self.nc.alloc_semaphore("D%d" % len(self.semh))
            self.dcnt[sname] = 0
        waits = self._deps(qeng, r, w)
        self.dcnt[sname] += 16
        tok = (sname, self.dcnt[sname])
        self.q[qeng].append((waits, (lambda e: e.dma_start(out=out, in_=in_)), (sname, 16)))
        self._commit(tok, r, w)
        return tok

    def wait_keys(self, eng, keys):
        waits = self._deps(eng, (), keys)
        self.q[eng].append((waits, None, None))

    def emit(self):
        nc = self.nc
        names = {"pe": "tensor", "act": "scalar", "dve": "vector", "pool": "gpsimd", "sp": "sync"}
        with nc.Block() as blk:
            for e in self.ENG:
                lst = self.q[e]
                if not lst:
                    continue

                def body(eng, lst=lst):
                    for waits, fn, inc in lst:
                        for s, v in waits:
                            eng.wait_ge(self.semh[s], v)
                        if fn is not None:
                            fn(eng).then_inc(self.semh[inc[0]], inc[1])
                getattr(blk, names[e])(body)


class Ring:
    def __init__(self, nc, name, shape, dtype, n, psum=False):
        self.n = n
        self.i = 0
        self.name = name
        al = nc.alloc_psum_tensor if psum else nc.alloc_sbuf_tensor
        self.t = [al("%s_%d" % (name, i), list(shape), dtype) for i in range(n)]

    def get(self):
        j = self.i % self.n
        self.i += 1
        return self.t[j], "%s_%d" % (self.name, j)


class Ctx:
    def __init__(self, nc, P):
        self.nc = nc
        self.P = P
        self.n = 0

    def sb(self, name, shape, dtype=F32):
        self.n += 1
        return self.nc.alloc_sbuf_tensor("%s_%d" % (name, self.n), list(shape), dtype)

    def mm(self, out, lhsT, rhs, start, stop, r, w):
        self.P.op("pe", lambda e: e.matmul(out, lhsT=lhsT, rhs=rhs, start=start, stop=stop), r=r, w=w)

    def tr(self, out, in_, ident, r, w):
        self.P.op("pe", lambda e: e.transpose(out, in_, ident), r=r, w=w)

    def act(self, out, in_, func, r, w, bias=None, scale=None, accum_out=None):
        kw = {}
        if bias is not None:
            kw["bias"] = bias
        if scale is not None:
            kw["scale"] = scale
        if accum_out is not None:
            kw["accum_out"] = accum_out
        self.P.op("act", lambda e: e.activation(out=out, in_=in_, func=func, **kw), r=r, w=w)

    def ts(self, out, in0, s1, s2, op0, op1, r, w, eng="dve"):
        if op1 is None:
            self.P.op(eng, lambda e: e.tensor_scalar(out=out, in0=in0, scalar1=s1, scalar2=None, op0=op0), r=r, w=w)
        else:
            self.P.op(eng, lambda e: e.tensor_scalar(out=out, in0=in0, scalar1=s1, scalar2=s2, op0=op0, op1=op1), r=r, w=w)

    def stt(self, out, in0, s, in1, op0, op1, r, w):
        self.P.op("dve", lambda e: e.scalar_tensor_tensor(out=out, in0=in0, scalar=s, in1=in1, op0=op0, op1=op1), r=r, w=w)

    def tt(self, out, in0, in1, op, r, w, eng="dve"):
        self.P.op(eng, lambda e: e.tensor_tensor(out=out, in0=in0, in1=in1, op=op), r=r, w=w)

    def cp(self, out, in_, r, w, eng="dve"):
        if eng == "act":
            self.P.op(eng, lambda e: e.activation(out=out, in_=in_, func=AF.Copy), r=r, w=w)
        else:
            self.P.op(eng, lambda e: e.tensor_copy(out=out, in_=in_), r=r, w=w)

    def recip(self, out, in_, r, w):
        self.P.op("dve", lambda e: e.reciprocal(out=out, in_=in_), r=r, w=w)

    def memset(self, ap, val, w, eng="pool"):
        self.P.op(eng, lambda e: e.memset(ap, val), r=(), w=w)


DK = 128
NFM = 1280
NTM = 772
PI = math.pi
PI_LO = 3.1415925


def build_mixer(nc, nblk=T // 512, stage=9):
    P = Prog(nc)
    C = Ctx(nc, P)
    uT = nc.dram_tensor("uT", [D, T], BF16, kind="ExternalInput").ap()
    wfm_d = nc.dram_tensor("wfm", [D, NFM], F32, kind="ExternalInput").ap()
    wtm_d = nc.dram_tensor("wtm", [D, NTM], F32, kind="ExternalInput").ap()
    cw_d = nc.dram_tensor("cw", [128, 24], F32, kind="ExternalInput").ap()
    small_d = nc.dram_tensor("small", [128, 8], F32, kind="ExternalInput").ap()
    retgn_d = nc.dram_tensor("retgn", [128, 256], F32, kind="ExternalInput").ap()
    gdngn_d = nc.dram_tensor("gdngn", [128, 128], F32, kind="ExternalInput").ap()
    goT = nc.dram_tensor("goT", [512, T], BF16, kind="ExternalOutput").ap()

    wfm = C.sb("wfm", [128, 8, NFM], BF16)
    wtm = C.sb("wtm", [128, 8, NTM], BF16)
    P.dma("pool", wfm[:], wfm_d.rearrange("(c p) n -> p c n", p=128), w=["wfm"])
    P.dma("pool", wtm[:], wtm_d.rearrange("(c p) n -> p c n", p=128), w=["wtm"])
    cw = C.sb("cw", [128, 24])
    small = C.sb("small", [128, 8])
    retgn = C.sb("retgn", [128, 256])
    gdngn = C.sb("gdngn", [128, 128])
    P.dma("sp", cw[:], cw_d, w=["cw"])
    P.dma("sp", small[:], small_d, w=["small"])
    P.dma("sp", retgn[:], retgn_d, w=["retgn"])
    P.dma("sp", gdngn[:], gdngn_d, w=["gdngn"])

    rel_i = C.sb("rel_i", [128, 128], I32)
    P.op("pool", lambda e: e.iota(rel_i[:], pattern=[[1, 128]], base=0, channel_multiplier=-1), w=["rel_i"])
    j_i = C.sb("j_i", [128, 128], I32)
    P.op("pool", lambda e: e.iota(j_i[:], pattern=[[1, 128]], base=0, channel_multiplier=0), w=["j_i"])
    p_i = C.sb("p_i", [128, 128], I32)
    P.op("pool", lambda e: e.iota(p_i[:], pattern=[[0, 128]], base=0, channel_multiplier=1), w=["p_i"])
    rel = C.sb("rel", [128, 128])
    Jf = C.sb("Jf", [128, 128])
    Pf = C.sb("Pf", [128, 128])
    C.cp(rel[:], rel_i[:], r=["rel_i"], w=["rel"])
    C.cp(Jf[:], j_i[:], r=["j_i"], w=["Jf"])
    C.cp(Pf[:], p_i[:], r=["p_i"], w=["Pf"])
    ident_f = C.sb("ident_f", [128, 128])
    ident_b = C.sb("ident_b", [128, 128], BF16)
    C.ts(ident_f[:], rel[:], 0.0, None, ALU.is_equal, None, r=["rel"], w=["ident_f"])
    C.cp(ident_b[:], ident_f[:], r=["ident_f"], w=["ident_b"])
    negones_b = C.sb("negones_b", [128, 128], BF16)
    ones_b = C.sb("ones_b", [128, 128], BF16)
    C.memset(negones_b[:], -1.0, w=["negones_b"])
    C.memset(ones_b[:], 1.0, w=["ones_b"])
    jc = C.sb("jc", [128, 128])
    pc = C.sb("pc", [128, 128])
    ge = C.sb("ge", [128, 128])
    BD = C.sb("BD", [128, 128])
    MU = C.sb("MU", [128, 128])
    ML = C.sb("ML", [128, 128])
    C.ts(jc[:], Jf[:], 64.0, None, ALU.is_ge, None, r=["Jf"], w=["jc"])
    C.ts(pc[:], Pf[:], 64.0, None, ALU.is_ge, None, r=["Pf"], w=["pc"])
    C.ts(ge[:], rel[:], 0.0, None, ALU.is_ge, None, r=["rel"], w=["ge"])
    C.tt(BD[:], jc[:], pc[:], ALU.is_equal, r=["jc", "pc"], w=["BD"])
    C.tt(MU[:], BD[:], ge[:], ALU.mult, r=["BD", "ge"], w=["MU"])
    C.tt(ML[:], BD[:], MU[:], ALU.subtract, r=["BD", "MU"], w=["ML"])
    MU_b = C.sb("MU_b", [128, 128], BF16)
    ML_b = C.sb("ML_b", [128, 128], BF16)
    C.cp(MU_b[:], MU[:], r=["MU"], w=["MU_b"])
    C.cp(ML_b[:], ML[:], r=["ML"], w=["ML_b"])
    csel = C.sb("csel", [128, 2])
    C.ts(csel[:, 0:1], pc[:, 0:1], -1.0, 1.0, ALU.mult, ALU.add, r=["pc"], w=["csel"])
    C.cp(csel[:, 1:2], pc[:, 0:1], r=["pc", "csel"], w=["csel"])
    LN2 = math.log(2.0)
    e1 = C.sb("e1", [128, 1])
    lg = C.sb("lg", [128, 1])
    C.act(e1[:], small[:, 0:1], AF.Exp, r=["small"], w=["e1"], scale=-LN2, bias=-5.0 * LN2)
    C.act(lg[:], e1[:], AF.Ln, r=["e1"], w=["lg"], scale=-1.0, bias=1.0)
    relpos = C.sb("relpos", [128, 128])
    C.ts(relpos[:], rel[:], 0.0, None, ALU.max, None, r=["rel"], w=["relpos"])
    dmT = C.sb("dmT", [128, 128])
    C.act(dmT[:], relpos[:], AF.Exp, r=["relpos", "lg"], w=["dmT"], scale=lg[:, 0:1])
    C.stt(dmT[:], dmT[:], DK ** -0.5, ge[:], ALU.mult, ALU.mult, r=["dmT", "ge"], w=["dmT"])
    decq = C.sb("decq", [128, 128])
    j1 = C.sb("j1", [128, 128])
    C.ts(j1[:], Jf[:], 1.0, None, ALU.add, None, r=["Jf"], w=["j1"])
    C.act(decq[:], j1[:], AF.Exp, r=["j1", "lg"], w=["decq"], scale=lg[:, 0:1])
    kdecs = C.sb("kdecs", [128, 1])
    t127 = C.sb("t127", [128, 1])
    C.ts(t127[:], Pf[:, 0:1], -1.0, 127.0, ALU.mult, ALU.add, r=["Pf"], w=["t127"])
    C.act(kdecs[:], t127[:], AF.Exp, r=["t127", "lg"], w=["kdecs"], scale=lg[:, 0:1])
    C.ts(kdecs[:], kdecs[:], DK ** -0.5, None, ALU.mult, None, r=["kdecs"], w=["kdecs"])
    Rdec = C.sb("Rdec", [128, 1])
    C.act(Rdec[:], lg[:], AF.Exp, r=["lg"], w=["Rdec"], scale=128.0)
    negA = C.sb("negA", [128, 2])
    C.act(negA[:], small[:, 1:3], AF.Exp, r=["small"], w=["negA"])
    C.ts(negA[:], negA[:], -1.0, None, ALU.mult, None, r=["negA"], w=["negA"])
    COS = C.sb("COS", [128, T], BF16)
    SINS = C.sb("SINS", [128, T], BF16)
    pm = C.sb("pm", [128, 1])
    fr = C.sb("fr", [128, 1])
    sgn = C.sb("sgn", [128, 1])
    C.stt(pm[:], pc[:, 0:1], -64.0, Pf[:, 0:1], ALU.mult, ALU.add, r=["pc", "Pf"], w=["pm"])
    C.act(fr[:], pm[:], AF.Exp, r=["pm"], w=["fr"], scale=-math.log(10000.0) / 64.0)
    C.ts(sgn[:], pc[:, 0:1], 2.0, -1.0, ALU.mult, ALU.add, r=["pc"], w=["sgn"])
    PW = 512
    t_i = C.sb("t_i", [128, PW], I32)
    ang = C.sb("ang", [128, PW])
    rr = C.sb("rr", [128, PW])
    n_i = C.sb("n_i", [128, PW], I32)
    msk = C.sb("msk", [128, PW])
    for pc_ in range(T // PW):
        P.op("pool", lambda e, b=pc_ * PW: e.iota(t_i[:], pattern=[[1, PW]], base=b, channel_multiplier=0), w=["t_i"])
        C.ts(ang[:], t_i[:], fr[:, 0:1], None, ALU.mult, None, r=["t_i", "fr"], w=["ang"])
        for phase, tab, tk_, sc in ((0.0, SINS, "SINS", sgn[:, 0:1]), (PI / 2, COS, "COS", 1.0)):
            C.ts(rr[:], ang[:], phase, 1.0 / (2 * PI), ALU.add, ALU.mult, r=["ang"], w=["rr"])
            C.cp(n_i[:], rr[:], r=["rr"], w=["n_i"])
            C.ts(rr[:], ang[:], phase, None, ALU.add, None, r=["ang", "n_i"], w=["rr"])
            C.stt(rr[:], n_i[:], -2 * PI, rr[:], ALU.mult, ALU.add, r=["n_i", "rr"], w=["rr"])
            C.ts(msk[:], rr[:], PI, None, ALU.is_gt, None, r=["rr"], w=["msk"])
            C.stt(rr[:], msk[:], -2 * PI, rr[:], ALU.mult, ALU.add, r=["msk", "rr"], w=["rr"])
            C.ts(msk[:], rr[:], -PI, None, ALU.is_lt, None, r=["rr"], w=["msk"])
            C.stt(rr[:], msk[:], 2 * PI, rr[:], ALU.mult, ALU.add, r=["msk", "rr"], w=["rr"])
            C.ts(rr[:], rr[:], PI_LO, -PI_LO, ALU.min, ALU.max, r=["rr"], w=["rr"])
            C.act(tab[:, pc_ * PW:(pc_ + 1) * PW], rr[:], AF.Sin, r=["rr", "sgn"], w=[tk_], scale=sc)

    R_f = C.sb("R_f", [128, 256])
    R_b = Ring(nc, "R_b", [128, 256], BF16, 2)
    S_f = [C.sb("S_f", [128, 128]) for _ in range(2)]
    S_b = [Ring(nc, "S_b%d" % h, [128, 128], BF16, 4) for h in range(2)]
    C.memset(R_f[:], 0.0, w=["R_f"])
    Rb_cur, Rb_key = R_b.get()
    C.memset(Rb_cur[:], 0.0, w=[Rb_key])
    Sb_cur = [None, None]
    for h in range(2):
        C.memset(S_f[h][:], 0.0, w=["S_f%d" % h])
        t_, k_ = S_b[h].get()
        C.memset(t_[:], 0.0, w=[k_])
        Sb_cur[h] = (t_, k_)
    pre = C.sb("pre", [128, 6, 515])
    C.memset(pre[:], 0.0, w=["pre%d" % c for c in range(6)])
    qd0 = [Ring(nc, "qd0_%d" % h, [128, 128], BF16, 2) for h in range(2)]
    qd1 = [Ring(nc, "qd1_%d" % h, [128, 128], BF16, 2) for h in range(2)]
    for h in range(2):
        for rg_ in (qd0[h], qd1[h]):
            for i in range(2):
                C.memset(rg_.t[i][:], 0.0, w=["%s_%d" % (rg_.name, i)])

    uring = Ring(nc, "ub", [128, 8, 512], BF16, 2)
    psA = Ring(nc, "psA", [128, 512], F32, 2, psum=True)
    psB = nc.alloc_psum_tensor("psB", [128, 1024], F32)
    psT = nc.alloc_psum_tensor("psT", [128, 1024], BF16)
    psQb = [nc.alloc_psum_tensor("psQ%d" % i, [128, 512], F32) for i in range(3)]
    qctr = [0]

    def psq():
        i = qctr[0] % 12
        qctr[0] += 1
        b_, s_ = i % 3, i // 3
        return psQb[b_][:, s_ * 128:(s_ + 1) * 128], "psQb_%d" % b_
    f512 = Ring(nc, "f512", [128, 512], F32, 6)
    b512 = Ring(nc, "b512", [128, 512], BF16, 4)
    qrT = Ring(nc, "qrT", [128, 512], BF16, 2)
    krT = Ring(nc, "krT", [128, 512], BF16, 2)
    nT = [Ring(nc, "nT%d" % c, [128, 512], BF16, 2) for c in range(6)]
    tk = Ring(nc, "tk", [128, 640], BF16, 2)
    rvb = Ring(nc, "rvb", [128, 256], BF16, 2)
    sg = Ring(nc, "sg", [128, 512], F32, 2)
    gb4 = Ring(nc, "gb4", [128, 4], F32, 2)
    f128 = Ring(nc, "f128", [128, 128], F32, 8)
    b128 = Ring(nc, "b128", [128, 128], BF16, 14)
    bd = Ring(nc, "bd", [128, 128], BF16, 8)
    ghl = Ring(nc, "ghl", [128, 4], BF16, 2)
    g2r = Ring(nc, "g2r", [128, 4], BF16, 3)
    f256 = Ring(nc, "f256", [128, 256], F32, 3)
    b256 = Ring(nc, "b256", [128, 256], BF16, 3)
    c8 = Ring(nc, "c8", [128, 8], F32, 8)
    gst = Ring(nc, "gst", [128, 4, 512], BF16, 2)
    uTv = uT.rearrange("(c p) t -> p c t", p=128)
    goTv = goT.rearrange("(s p) t -> p s t", p=128)

    def load_u(tb):
        ub, ubk = uring.get()
        P.dma("sp", ub[:], uTv[:, :, tb * 512:(tb + 1) * 512], w=[ubk])
        return ub, ubk

    nxt = load_u(0)
    gst_keys = set()
    for tb in range(nblk if stage > 0 else 0):
        ub, ubk = nxt
        if tb + 1 < nblk:
            nxt = load_u(tb + 1)
        c0 = tb * 512
        gs, gsk = gst.get()
        gst_keys.add(gsk)
        qr, qrk = qrT.get()
        kr, krk = krT.get()
        for m in range(10):
            ps, psk = psA.get()
            for kc in range(8):
                C.mm(ps[:], wfm[:, kc, m * 128:(m + 1) * 128], ub[:, kc, :], kc == 0, kc == 7, r=["wfm", ubk], w=[psk])
            if m in (0, 2):
                t0, t0k = f512.get()
                C.tt(t0[:], ps[:], COS[:, c0:c0 + 512], ALU.mult, r=[psk, "COS"], w=[t0k])
            elif m in (1, 3):
                t1, t1k = f512.get()
                C.tt(t1[:], ps[:], SINS[:, c0:c0 + 512], ALU.mult, r=[psk, "SINS"], w=[t1k])
                dst, dk_ = (qr, qrk) if m == 1 else (kr, krk)
                C.tt(dst[:], t0[:], t1[:], ALU.add, r=[t0k, t1k], w=[dk_])
            else:
                c = m - 4
                C.act(pre[:, c, 3:515], ps[:], AF.Copy, r=[psk], w=["pre%d" % c])
        nTt = []
        for c in range(6):
            acc, acck = f512.get()
            pk = "pre%d" % c
            C.ts(acc[:], pre[:, c, 0:512], cw[:, c * 4:c * 4 + 1], None, ALU.mult, None, r=[pk, "cw"], w=[acck])
            for w_ in range(1, 4):
                C.stt(acc[:], pre[:, c, w_:w_ + 512], cw[:, c * 4 + w_:c * 4 + w_ + 1], acc[:], ALU.mult, ALU.add,
                      r=[pk, "cw", acck], w=[acck])
            C.cp(pre[:, c, 0:3], pre[:, c, 512:515], r=[pk], w=[pk], eng="pool")
            o_, ok_ = nT[c].get()
            if c >= 4:
                C.act(o_[:], acc[:], AF.Silu, r=[acck], w=[ok_])
            else:
                post, postk = f512.get()
                C.act(post[:], acc[:], AF.Silu, r=[acck], w=[postk])
                sq, sqk = b512.get()
                C.act(sq[:], post[:], AF.Square, r=[postk], w=[sqk])
                ps, psk = psA.get()
                C.mm(ps[:], ones_b[:], sq[:], True, True, r=["ones_b", sqk], w=[psk])
                rs, rsk = f512.get()
                C.act(rs[:], ps[:], AF.Sqrt, r=[psk], w=[rsk], bias=EPS)
                C.recip(rs[:], rs[:], r=[rsk], w=[rsk])
                if c < 2:
                    C.stt(o_[:], post[:], DK ** -0.5, rs[:], ALU.mult, ALU.mult, r=[postk, rsk], w=[ok_])
                else:
                    C.tt(o_[:], post[:], rs[:], ALU.mult, r=[postk, rsk], w=[ok_])
            nTt.append((o_, ok_))
        for ti in range(4 if stage > 1 else 0):
            cs = slice(ti * 128, (ti + 1) * 128)
            for kc in range(8):
                C.mm(psB[:, 0:512], ub[:, kc, cs], wtm[:, kc, 0:512], kc == 0, kc == 7, r=[ubk, "wtm"], w=["psB0"])
            for kc in range(8):
                C.mm(psB[:, 512:772], ub[:, kc, cs], wtm[:, kc, 512:772], kc == 0, kc == 7, r=[ubk, "wtm"], w=["psB1"])
            rv, rvk = rvb.get()
            C.cp(rv[:], psB[:, 0:256], r=["psB0"], w=[rvk])
            sgt, sgk = sg.get()
            C.act(sgt[:, 0:256], psB[:, 256:512], AF.Silu, r=["psB0"], w=[sgk])
            C.act(sgt[:, 256:512], psB[:, 512:768], AF.Silu, r=["psB1", sgk], w=[sgk])
            g4, g4k = gb4.get()
            xx, xxk = c8.get()
            C.tt(xx[:, 0:2], psB[:, 768:770], small[:, 3:5], ALU.add, r=["psB1", "small"], w=[xxk])
            C.act(xx[:, 0:2], xx[:, 0:2], AF.Exp, r=[xxk], w=[xxk])
            C.act(xx[:, 0:2], xx[:, 0:2], AF.Ln, r=[xxk], w=[xxk], bias=1.0)
            C.tt(g4[:, 0:2], xx[:, 0:2], negA[:], ALU.mult, r=[xxk, "negA"], w=[g4k])
            C.act(g4[:, 2:4], psB[:, 770:772], AF.Sigmoid, r=["psB1", g4k], w=[g4k])
            gh, ghk = ghl.get()
            C.cp(gh[:, 0:2], g4[:, 0:2], r=[g4k], w=[ghk])
            C.tt(gh[:, 2:4], g4[:, 0:2], gh[:, 0:2], ALU.subtract, r=[g4k, ghk], w=[ghk])
            srcs = [(kr, krk)] + [nTt[2], nTt[3], nTt[4], nTt[5]]
            for i, (s_, sk_) in enumerate(srcs):
                C.tr(psT[:, i * 128:(i + 1) * 128], s_[:, cs], ident_b[:], r=[sk_, "ident_b"], w=["psT"])
            tkt, tkk = tk.get()
            C.cp(tkt[:], psT[:, 0:640], r=["psT"], w=[tkk])
            if stage < 3:
                continue
            sc, sck = psq()
            C.mm(sc, kr[:, cs], qr[:, cs], True, True, r=[krk, qrk], w=[sck])
            scT, scTk = b128.get()
            C.tt(scT[:], sc, dmT[:], ALU.mult, r=[sck, "dmT"], w=[scTk])
            qd, qdk = b128.get()
            C.tt(qd[:], qr[:, cs], decq[:], ALU.mult, r=[qrk, "decq"], w=[qdk], eng="pool")
            ops_, opsk = psA.get()
            C.mm(ops_[:, 0:256], scT[:], rv[:], True, False, r=[scTk, rvk], w=[opsk])
            C.mm(ops_[:, 0:256], qd[:], Rb_cur[:], False, True, r=[qdk, Rb_key], w=[opsk])
            kd, kdk = b128.get()
            C.ts(kd[:], tkt[:, 0:128], kdecs[:, 0:1], None, ALU.mult, None, r=[tkk, "kdecs"], w=[kdk], eng="pool")
            dR, dRk = psA.get()
            C.mm(dR[:, 0:256], kd[:], rv[:], True, True, r=[kdk, rvk], w=[dRk])
            C.stt(R_f[:], R_f[:], Rdec[:, 0:1], dR[:, 0:256], ALU.mult, ALU.add, r=["R_f", "Rdec", dRk], w=["R_f"])
            Rb_cur, Rb_key = R_b.get()
            C.cp(Rb_cur[:], R_f[:], r=["R_f"], w=[Rb_key], eng="act")
            st6, st6k = c8.get()
            P.op("dve", lambda e, a=st6, b=ops_: e.bn_stats(out=a[:, 0:6], in_=b[:, 0:256]), r=[opsk], w=[st6k])
            mv, mvk = c8.get()
            P.op("dve", lambda e, a=mv, b=st6: e.bn_aggr(out=a[:, 0:2], in_=b[:, 0:6]), r=[st6k], w=[mvk])
            C.act(mv[:, 2:3], mv[:, 1:2], AF.Sqrt, r=[mvk], w=[mvk], bias=EPS)
            C.recip(mv[:, 2:3], mv[:, 2:3], r=[mvk], w=[mvk])
            on, onk = f256.get()
            C.ts(on[:], ops_[:, 0:256], mv[:, 0:1], mv[:, 2:3], ALU.subtract, ALU.mult, r=[opsk, mvk], w=[onk])
            C.tt(on[:], on[:], retgn[:], ALU.mult, r=[onk, "retgn"], w=[onk], eng="pool")
            go, gok = b256.get()
            C.tt(go[:], on[:], sgt[:, 0:256], ALU.mult, r=[onk, sgk], w=[gok])
            for hf in range(2):
                C.tr(psT[:, 640 + hf * 128:640 + (hf + 1) * 128], go[:, hf * 128:(hf + 1) * 128], ident_b[:],
                     r=[gok, "ident_b"], w=["psT"])
            C.cp(gs[:, 0, cs], psT[:, 640:768], r=["psT"], w=[gsk], eng="act")
            C.cp(gs[:, 1, cs], psT[:, 768:896], r=["psT", gsk], w=[gsk], eng="act")
            for hh in range(2 if stage > 3 else 0):
                gcol = g4[:, hh:hh + 1]
                beta = g4[:, 2 + hh:3 + hh]
                qn, qnk = nTt[hh]
                kn, knk = nTt[2 + hh]
                kn_tok = tkt[:, 128 + hh * 128:256 + hh * 128]
                v_tok = tkt[:, 384 + hh * 128:512 + hh * 128]
                Lg, Lgk = b128.get()
                C.ts(Lg[:], MU[:], gcol, None, ALU.mult, None, r=["MU", g4k], w=[Lgk])
                Ll, Llk = b128.get()
                C.stt(Ll[:], MU[:], gcol, Lg[:], ALU.mult, ALU.subtract, r=["MU", g4k, Lgk], w=[Llk])
                Dp, Dpk = psq()
                C.mm(Dp, Lg[:], ones_b[:], True, False, r=[Lgk, "ones_b"], w=[Dpk])
                C.mm(Dp, Ll[:], ones_b[:], False, False, r=[Llk, "ones_b"], w=[Dpk])
                C.mm(Dp, negones_b[:], Lg[:], False, False, r=[Lgk, "negones_b"], w=[Dpk])
                C.mm(Dp, negones_b[:], Ll[:], False, True, r=[Llk, "negones_b"], w=[Dpk])
                Y, Yk = f128.get()
                C.ts(Y[:], Dp, 0.0, None, ALU.min, None, r=[Dpk], w=[Yk])
                C.act(Y[:], Y[:], AF.Exp, r=[Yk], w=[Yk])
                X, Xk = f128.get()
                C.ts(X[:], Dp, 0.0, -1.0, ALU.max, ALU.mult, r=[Dpk], w=[Xk])
                C.act(X[:], X[:], AF.Exp, r=[Xk], w=[Xk])
                gcB, gcBk = psq()
                C.mm(gcB, ones_b[:], Lg[:], True, False, r=["ones_b", Lgk], w=[gcBk])
                C.mm(gcB, ones_b[:], Ll[:], False, True, r=["ones_b", Llk], w=[gcBk])
                egcB, egcBk = f128.get()
                C.act(egcB[:], gcB, AF.Exp, r=[gcBk], w=[egcBk])
                g2, g2k = g2r.get()
                C.ts(g2[:, 0:2], csel[:], gh[:, hh:hh + 1], None, ALU.mult, None, r=["csel", ghk], w=[g2k])
                C.ts(g2[:, 2:4], csel[:], gh[:, 2 + hh:3 + hh], None, ALU.mult, None, r=["csel", ghk, g2k], w=[g2k])
                p4, p4k = psq()
                C.mm(p4[:, 0:1], MU_b[:], gh[:, hh:hh + 1], True, False, r=["MU_b", ghk], w=[p4k])
                C.mm(p4[:, 0:1], MU_b[:], gh[:, 2 + hh:3 + hh], False, True, r=["MU_b", ghk], w=[p4k])
                C.mm(p4[:, 1:2], ML_b[:], gh[:, hh:hh + 1], True, False, r=["ML_b", ghk], w=[p4k])
                C.mm(p4[:, 1:2], ML_b[:], gh[:, 2 + hh:3 + hh], False, True, r=["ML_b", ghk], w=[p4k])
                C.mm(p4[:, 2:4], ones_b[:], g2[:, 0:2], True, False, r=["ones_b", g2k], w=[p4k])
                C.mm(p4[:, 2:4], ones_b[:], g2[:, 2:4], False, True, r=["ones_b", g2k], w=[p4k])
                e4, e4k = c8.get()
                C.act(e4[:, 0:4], p4[:, 0:4], AF.Exp, r=[p4k], w=[e4k])
                C.tt(e4[:, 4:5], beta, e4[:, 0:1], ALU.mult, r=[g4k, e4k], w=[e4k])
                if stage < 5:
                    continue
                kbg, kbgk = b128.get()
                C.ts(kbg[:], kn_tok, e4[:, 4:5], None, ALU.mult, None, r=[tkk, e4k], w=[kbgk], eng="pool")
                vb, vbk = b128.get()
                C.ts(vb[:], v_tok, beta, None, ALU.mult, None, r=[tkk, g4k], w=[vbk], eng="pool")
                kdg, kdgk = b128.get()
                C.ts(kdg[:], kn_tok, e4[:, 1:2], None, ALU.mult, None, r=[tkk, e4k], w=[kdgk], eng="pool")
                q0, q0k = qd0[hh].get()
                q1, q1k = qd1[hh].get()
                C.tt(q0[:, 0:64], qn[:, ti * 128:ti * 128 + 64], egcB[:, 0:64], ALU.mult, r=[qnk, egcBk], w=[q0k])
                C.tt(q1[:, 64:128], qn[:, ti * 128 + 64:ti * 128 + 128], egcB[:, 64:128], ALU.mult, r=[qnk, egcBk], w=[q1k])
                Gp, Gpk = psq()
                C.mm(Gp, kn[:, cs], kn[:, cs], True, True, r=[knk], w=[Gpk])
                KQ, KQk = psq()
                C.mm(KQ, kn[:, cs], qn[:, cs], True, True, r=[knk, qnk], w=[KQk])
                C.tt(Y[:], Y[:], ML[:], ALU.mult, r=[Yk, "ML"], w=[Yk])
                A_, Ak = bd.get()
                C.stt(A_[:], Gp, beta, Y[:], ALU.mult, ALU.mult, r=[Gpk, g4k, Yk], w=[Ak])
                C.tt(X[:], X[:], MU[:], ALU.mult, r=[Xk, "MU"], w=[Xk], eng="pool")
                qkT, qkTk = b128.get()
                C.tt(qkT[:], KQ, X[:], ALU.mult, r=[KQk, Xk], w=[qkTk])
                if stage < 6:
                    continue
                Atq, Atpk = psq()
                Atp = Atq.bitcast(BF16)[:, 0:128]
                C.tr(Atp, A_[:], ident_b[:], r=[Ak, "ident_b"], w=[Atpk])
                Bm, Bk = bd.get()
                C.cp(Bm[:], Atp, r=[Atpk], w=[Bk], eng="act")
                Z, Zk = bd.get()
                C.tt(Z[:], ident_f[:], Atp, ALU.subtract, r=["ident_f", Atpk], w=[Zk])
                Pm, Pk, Ptm, Ptk = Bm, Bk, A_, Ak
                for lvl in range(5):
                    pt2p, pt2pk = psq()
                    C.mm(pt2p, Pm[:], Ptm[:], True, True, r=[Pk, Ptk], w=[pt2pk])
                    Pt2, Pt2k = bd.get()
                    C.cp(Pt2[:], pt2p, r=[pt2pk], w=[Pt2k])
                    if lvl < 4:
                        p2p, p2pk = psq()
                        C.mm(p2p, Ptm[:], Pm[:], True, True, r=[Pk, Ptk], w=[p2pk])
                        P2, P2k = bd.get()
                        C.cp(P2[:], p2p, r=[p2pk], w=[P2k], eng="act")
                    zp, zpk = psq()
                    C.mm(zp, Pt2[:], Z[:], True, True, r=[Pt2k, Zk], w=[zpk])
                    Zn, Znk = bd.get()
                    C.tt(Zn[:], zp, Z[:], ALU.add, r=[zpk, Zk], w=[Znk])
                    Z, Zk = Zn, Znk
                    if lvl < 4:
                        Pm, Pk = P2, P2k
                    Ptm, Ptk = Pt2, Pt2k
                if stage < 7:
                    continue
                Zb, Zbk = Z, Zk
                wTp, wTpk = psq()
                C.mm(wTp, kbg[:], Zb[:], True, True, r=[kbgk, Zbk], w=[wTpk])
                nwT, nwTk = b128.get()
                C.ts(nwT[:], wTp, -1.0, None, ALU.mult, None, r=[wTpk], w=[nwTk])
                vn, vnk = b128.get()
                S0, S0k = Sb_cur[hh]
                sfk = "S_f%d" % hh
                vp, vpk = psq()
                C.mm(vp, Zb[:], vb[:], True, False, r=[Zbk, vbk], w=[vpk])
                C.mm(vp, nwT[:], S0[:], False, True, r=[nwTk, S0k], w=[vpk])
                C.cp(vn[0:64, :], vp[0:64, :], r=[vpk], w=[vnk])
                dS, dSk = psq()
                C.mm(dS, kdg[0:64, :], vn[0:64, :], True, True, r=[kdgk, vnk], w=[dSk])
                C.stt(S_f[hh][:], S_f[hh][:], e4[:, 2:3], dS, ALU.mult, ALU.add, r=[sfk, e4k, dSk], w=[sfk])
                S1, S1k = S_b[hh].get()
                C.cp(S1[:], S_f[hh][:], r=[sfk], w=[S1k], eng="act")
                if stage < 8:
                    continue
                vp2, vp2k = psq()
                C.mm(vp2, Zb[:], vb[:], True, False, r=[Zbk, vbk], w=[vp2k])
                C.mm(vp2, nwT[:], S1[:], False, True, r=[nwTk, S1k], w=[vp2k])
                C.cp(vn[64:128, :], vp2[64:128, :], r=[vp2k, vnk], w=[vnk])
                dS2, dS2k = psq()
                C.mm(dS2, kdg[64:128, :], vn[64:128, :], True, True, r=[kdgk, vnk], w=[dS2k])
                op_, opk = psq()
                C.mm(op_, q0[:], S0[:], True, False, r=[q0k, S0k], w=[opk])
                C.mm(op_, q1[:], S1[:], False, False, r=[q1k, S1k], w=[opk])
                C.mm(op_, qkT[:], vn[:], False, True, r=[qkTk, vnk], w=[opk])
                C.stt(S_f[hh][:], S_f[hh][:], e4[:, 3:4], dS2, ALU.mult, ALU.add, r=[sfk, e4k, dS2k], w=[sfk])
                S2, S2k = S_b[hh].get()
                C.cp(S2[:], S_f[hh][:], r=[sfk], w=[S2k], eng="act")
                Sb_cur[hh] = (S2, S2k)
                if stage < 9:
                    continue
                junk, junkk = f128.get()
                ss, ssk = c8.get()
                C.act(junk[:], op_, AF.Square, r=[opk], w=[junkk])
                P.op("dve", lambda e, a=ss, b=junk: e.tensor_reduce(out=a[:, 0:1], in_=b[:], axis=mybir.AxisListType.X, op=ALU.add), r=[junkk], w=[ssk])
                C.act(ss[:, 1:2], ss[:, 0:1], AF.Sqrt, r=[ssk], w=[ssk], scale=1.0 / 128.0, bias=EPS)
                C.recip(ss[:, 1:2], ss[:, 1:2], r=[ssk], w=[ssk])
                og, ogk = f128.get()
                C.stt(og[:], op_, ss[:, 1:2], gdngn[:], ALU.mult, ALU.mult, r=[opk, ssk, "gdngn"], w=[ogk])
                gg, ggk = b128.get()
                C.tt(gg[:], og[:], sgt[:, 256 + hh * 128:384 + hh * 128], ALU.mult, r=[ogk, sgk], w=[ggk])
                C.tr(psT[:, 896:1024], gg[:], ident_b[:], r=[ggk, "ident_b"], w=["psT"])
                C.cp(gs[:, 2 + hh, cs], psT[:, 896:1024], r=["psT", gsk], w=[gsk], eng="act")
        P.dma("sp", goTv[:, :, c0:c0 + 512], gs[:], r=[gsk], slot=gsk)
    P.wait_keys("sp", sorted(gst_keys))
    P.emit()
    return nc


O_RQ, O_RK, O_RV, O_RG, O_GQ = 0, 512, 1024, 2048, 3072
O_GK, O_GV, O_GZ, O_GA, O_GB = 4096, 5120, 6144, 7168, 7176
O_MGR, O_MGG = 7184, 8208


def _swap_halves(w):
    return np.concatenate([w[:, 64:128], w[:, 0:64]], axis=1)


def mixer_inputs(layer, hg, w_in, conv_w, a_log, dt_bias, ret_gn, gdn_gn):
    W = w_in[layer]
    rq = W[:, O_RQ + hg * 128:O_RQ + (hg + 1) * 128]
    rk = W[:, O_RK + hg * 128:O_RK + (hg + 1) * 128]
    gq = W[:, O_GQ + hg * 256:O_GQ + (hg + 1) * 256]
    gk = W[:, O_GK + hg * 256:O_GK + (hg + 1) * 256]
    gv = W[:, O_GV + hg * 256:O_GV + (hg + 1) * 256]
    wfm = np.ascontiguousarray(np.concatenate([rq, _swap_halves(rq), rk, _swap_halves(rk), gq, gk, gv], axis=1))
    rv = W[:, O_RV + hg * 256:O_RV + (hg + 1) * 256]
    rg = W[:, O_RG + hg * 256:O_RG + (hg + 1) * 256]
    gz = W[:, O_GZ + hg * 256:O_GZ + (hg + 1) * 256]
    ga = W[:, O_GA + hg * 2:O_GA + hg * 2 + 2]
    gb = W[:, O_GB + hg * 2:O_GB + hg * 2 + 2]
    wtm = np.ascontiguousarray(np.concatenate([rv, rg, gz, ga, gb], axis=1))
    cwl = conv_w[layer]
    chunks = []
    for base in (0, 1024, 2048):
        for h in range(2):
            c0 = base + (2 * hg + h) * 128
            chunks.append(cwl[:, c0:c0 + 128].T)
    cw = np.ascontiguousarray(np.concatenate(chunks, axis=1)).astype(np.float32)
    small = np.zeros((128, 8), np.float32)
    small[:, 0] = np.float32(hg)
    small[:, 1:3] = a_log[layer, 2 * hg:2 * hg + 2][None, :]
    small[:, 3:5] = dt_bias[layer, 2 * hg:2 * hg + 2][None, :]
    retgn = np.ascontiguousarray(np.broadcast_to(ret_gn[layer, hg * 256:(hg + 1) * 256][None, :], (128, 256))).astype(np.float32)
    gdngn = np.ascontiguousarray(np.broadcast_to(gdn_gn[layer][None, :], (128, 128))).astype(np.float32)
    return {"wfm": wfm, "wtm": wtm, "cw": cw, "small": small, "retgn": retgn, "gdngn": gdngn}


NTB = TOK // 512


def build_token(nc, pre, ffn, post):
    P = Prog(nc)
    C = Ctx(nc, P)
    dt_in = lambda name, shape, dt=F32: nc.dram_tensor(name, list(shape), dt, kind="ExternalInput").ap()
    dt_out = lambda name, shape, dt=F32: nc.dram_tensor(name, list(shape), dt, kind="ExternalOutput").ap()
    hT_d = dt_in("hT", [D, TOK])
    norms_d = dt_in("norms", [128, 40])
    hT = C.sb("hT", [128, 8, TOK])
    uT = C.sb("uT", [128, 8, TOK], BF16)
    arena = C.sb("arena", [128, 16 * TOK], BF16)
    norms = C.sb("norms", [128, 40])
    ones_b = C.sb("ones_b", [128, 128], BF16)
    C.memset(ones_b[:], 1.0, w=["ones_b"], eng="dve")
    P.dma("sp", norms[:], norms_d, w=["norms"])
    hk = lambda c, tb: "h%d_%d" % (c, tb)
    uk = lambda c, tb: "u%d_%d" % (c, tb)
    ak = lambda c, tb: "ar%d_%d" % (c, tb)
    allk = lambda f, n: [f(c, tb) for c in range(n) for tb in range(NTB)]
    hv = hT_d.rearrange("(c p) t -> p c t", p=128)
    for c in range(8):
        P.dma("sp", hT[:, c, :], hv[:, c, :], w=[hk(c, tb) for tb in range(NTB)], slot="hload%d" % c)
    wst = Ring(nc, "wst", [128, 4096], BF16, 3)
    tmp = Ring(nc, "tmp", [128, 512], F32, 3)
    rstdr = Ring(nc, "rstd", [128, 512], F32, 2)
    psR = Ring(nc, "psR", [128, 512], F32, 6, psum=True)
    psM = Ring(nc, "psM", [128, 512], F32, 2, psum=True)
    A3 = arena[:].rearrange("p (c t) -> p c t", c=16)

    def tbs(tb):
        return slice(tb * 512, (tb + 1) * 512)

    def linear(pairs, M, consume):
        G = 512
        for (_, _, _, KC) in pairs:
            G = min(G, (4096 // KC) // 128 * 128)
        for g0 in range(0, M, G):
            gw = min(G, M - g0)
            staged = []
            for (w, x, xkf, KC) in pairs:
                wt, wk = wst.get()
                wv = wt[:, 0:KC * gw].rearrange("p (c m) -> p c m", c=KC)
                P.dma("pool", wv, w.rearrange("(c p) m -> p c m", p=128)[:, :, g0:g0 + gw], w=[wk])
                staged.append((wv, wk))
            for mi in range(gw // 128):
                mc = g0 // 128 + mi
                for tb in range(NTB):
                    pss = []
                    for (wv, wk), (w, x, xkf, KC) in zip(staged, pairs):
                        ps, psk = psR.get()
                        for kc in range(KC):
                            C.mm(ps[:], wv[:, kc, mi * 128:(mi + 1) * 128], x[:, kc, tbs(tb)], kc == 0, kc == KC - 1,
                                 r=[wk, xkf(kc, tb)], w=[psk])
                        pss.append((ps, psk))
                    consume(mc, tb, pss)

    def rmsnorm(gcol0, out, okf, router=None):
        for tb in range(NTB):
            ps, psk = psM.get()
            for c in range(8):
                sqf, sqk = tmp.get()
                sq = sqf[:].bitcast(BF16)[:, 0:512]
                C.act(sq, hT[:, c, tbs(tb)], AF.Square, r=[hk(c, tb)], w=[sqk])
                C.mm(ps[:], ones_b[:], sq, c == 0, c == 7, r=["ones_b", sqk], w=[psk])
            rs, rsk = rstdr.get()
            C.act(rs[:], ps[:], AF.Sqrt, r=[psk], w=[rsk], scale=1.0 / D, bias=EPS)
            C.recip(rs[:], rs[:], r=[rsk], w=[rsk])
            for c in range(8):
                C.stt(out[:, c, tbs(tb)], hT[:, c, tbs(tb)], norms[:, gcol0 + c:gcol0 + c + 1], rs[:], ALU.mult, ALU.mult,
                      r=[hk(c, tb), "norms", rsk], w=[okf(c, tb)])
            if router is not None:
                router(tb, rs, rsk, gcol0)

    if pre:
        goT_d = dt_in("goT", [2 * D, TOK], BF16)
        w_mg = dt_in("w_mg", [D, 2 * D])
        w_brr = dt_in("w_brr", [D, D])
        w_brg = dt_in("w_brg", [D, D])
        w_out = dt_in("w_out", [D, D])
        w_ple = dt_in("w_ple", [PLE, D])
        w_pleg = dt_in("w_pleg", [D, D])
        pT_d = dt_in("pT", [PLE, TOK])
        rmsnorm(0, uT, uk)
        gov = goT_d.rearrange("(c p) t -> p c t", p=128)
        for br in range(2):
            for c in range(8):
                P.dma("sp", A3[:, c, :], gov[:, br * 8 + c, :], w=[ak(c, tb) for tb in range(NTB)], slot="goload%d" % c)
            wb = w_brr if br == 0 else w_brg

            def cons(mc, tb, pss, br=br):
                (p1, p1k), (p2, p2k) = pss
                sg_, sgk_ = tmp.get()
                C.act(sg_[:], p2[:], AF.Sigmoid, r=[p2k], w=[sgk_])
                if br == 0:
                    C.tt(A3[:, 8 + mc, tbs(tb)], sg_[:], p1[:], ALU.mult, r=[sgk_, p1k], w=[ak(8 + mc, tb)])
                else:
                    C.tt(sg_[:], sg_[:], p1[:], ALU.mult, r=[sgk_, p1k], w=[sgk_])
                    C.tt(A3[:, 8 + mc, tbs(tb)], A3[:, 8 + mc, tbs(tb)], sg_[:], ALU.add, r=[sgk_, ak(8 + mc, tb)], w=[ak(8 + mc, tb)])
            linear([(wb, A3, ak, 8), (w_mg[:, br * D:(br + 1) * D], uT, uk, 8)], D, cons)

        def cons_res(mc, tb, pss):
            (p1, p1k), = pss
            C.tt(hT[:, mc, tbs(tb)], hT[:, mc, tbs(tb)], p1[:], ALU.add, r=[hk(mc, tb), p1k], w=[hk(mc, tb)])
        A3m = A3[:, 8:16, :]
        linear([(w_out, A3m, lambda c, tb: ak(8 + c, tb), 8)], D, cons_res)

        if ffn == "dense":
            wg = dt_in("wg", [D, DFF])
            wu = dt_in("wu", [D, DFF])
            wd = dt_in("wd", [DFF, D])
            rmsnorm(8, uT, uk)
            for half in range(2):
                def cons_a(mc, tb, pss):
                    (p1, p1k), (p2, p2k) = pss
                    sg_, sgk_ = tmp.get()
                    C.act(sg_[:], p1[:], AF.Silu, r=[p1k], w=[sgk_])
                    C.tt(A3[:, mc, tbs(tb)], sg_[:], p2[:], ALU.mult, r=[sgk_, p2k], w=[ak(mc, tb)])
                hs = slice(half * DFE, (half + 1) * DFE)
                linear([(wg[:, hs], uT, uk, 8), (wu[:, hs], uT, uk, 8)], DFE, cons_a)
                linear([(wd[hs, :], A3, ak, 11)], D, cons_res)
        else:
            router_d = dt_in("router", [D, NEXP])
            ewg = dt_in("ewg", [NEXP, D, DFE])
            ewu = dt_in("ewu", [NEXP, D, DFE])
            ewd = dt_in("ewd", [NEXP, DFE, D])
            rt = C.sb("rt", [128, 8, NEXP])
            P.dma("sp", rt[:], router_d.rearrange("(c p) e -> p c e", p=128), w=["rt"])
            rth = C.sb("rth", [128, 8, NEXP], BF16)
            rtl = C.sb("rtl", [128, 8, NEXP], BF16)
            C.cp(rth[:], rt[:], r=["rt"], w=["rth"])
            C.tt(rtl[:], rt[:], rth[:], ALU.subtract, r=["rt", "rth"], w=["rtl"])
            ident_b = C.sb("ident_b", [128, 128], BF16)
            rel_i = C.sb("rel_i", [128, 128], I32)
            P.op("pool", lambda e: e.iota(rel_i[:], pattern=[[1, 128]], base=0, channel_multiplier=-1), w=["rel_i"])
            C.ts(ident_b[:], rel_i[:], 0.0, None, ALU.is_equal, None, r=["rel_i"], w=["ident_b"])
            sel_i = C.sb("sel_i", [16, 128], I32)
            sel = C.sb("sel", [16, NEXP * 128], BF16)
            P.op("pool", lambda e: e.iota(sel_i[:], pattern=[[0, 128]], base=0, channel_multiplier=-1), w=["sel_i"])
            for e_ in range(NEXP):
                C.ts(sel[:, e_ * 128:(e_ + 1) * 128], sel_i[:], float(-e_), None, ALU.is_equal, None, r=["sel_i", "sel"], w=["sel"])
                C.stt(sel[:, e_ * 128:(e_ + 1) * 128], sel_i[:], float(-(e_ + 8)), sel[:, e_ * 128:(e_ + 1) * 128],
                      ALU.is_equal, ALU.add, r=["sel_i", "sel"], w=["sel"])
            gT = arena[0:16, 12 * TOK:13 * TOK]
            gbc = arena[:, 14 * TOK:16 * TOK].bitcast(F32)
            uf = Ring(nc, "uf", [128, 128], F32, 2)
            ufb = Ring(nc, "ufb", [128, 256], BF16, 3)
            c8 = Ring(nc, "c8r", [128, 8], F32, 12)
            c16 = Ring(nc, "c16r", [128, 16], BF16, 2)

            def router(tb, rs, rsk, gcol0):
                for ti in range(4):
                    cs = slice(tb * 512 + ti * 128, tb * 512 + (ti + 1) * 128)
                    lp, lpk = psM.get()
                    for c in range(8):
                        u_, u_k = uf.get()
                        C.stt(u_[:], hT[:, c, cs], norms[:, gcol0 + c:gcol0 + c + 1], rs[:, ti * 128:(ti + 1) * 128],
                              ALU.mult, ALU.mult, r=[hk(c, tb), "norms", rsk], w=[u_k])
                        ub_, ub_k = ufb.get()
                        C.cp(ub_[:, 0:128], u_[:], r=[u_k], w=[ub_k])
                        C.tt(ub_[:, 128:256], u_[:], ub_[:, 0:128], ALU.subtract, r=[u_k, ub_k], w=[ub_k])
                        C.mm(lp[:, 0:NEXP], ub_[:, 0:128], rth[:, c, :], c == 0, False, r=[ub_k, "rth"], w=[lpk])
                        C.mm(lp[:, 0:NEXP], ub_[:, 128:256], rth[:, c, :], False, False, r=[ub_k, "rth"], w=[lpk])
                        C.mm(lp[:, 0:NEXP], ub_[:, 0:128], rtl[:, c, :], False, c == 7, r=[ub_k, "rtl"], w=[lpk])
                    lg_, lgk = c8.get()
                    C.cp(lg_[:], lp[:, 0:NEXP], r=[lpk], w=[lgk])
                    m8, m8k = c8.get()
                    P.op("dve", lambda e, a=m8, b=lg_: e.max(out=a[:], in_=b[:]), r=[lgk], w=[m8k])
                    nm, nmk = c8.get()
                    C.ts(nm[:, 0:1], m8[:, 0:1], -1.0, None, ALU.mult, None, r=[m8k], w=[nmk])
                    mk_, mkk = c8.get()
                    C.ts(mk_[:], lg_[:], m8[:, 1:2], None, ALU.is_ge, None, r=[lgk, m8k], w=[mkk])
                    ex, exk = c8.get()
                    C.act(ex[:], lg_[:], AF.Exp, r=[lgk, nmk], w=[exk], bias=nm[:, 0:1])
                    C.tt(ex[:], ex[:], mk_[:], ALU.mult, r=[exk, mkk], w=[exk])
                    P.op("dve", lambda e, a=nm, b=ex: e.tensor_reduce(out=a[:, 1:2], in_=b[:], axis=mybir.AxisListType.X, op=ALU.add),
                         r=[exk, nmk], w=[nmk])
                    C.recip(nm[:, 1:2], nm[:, 1:2], r=[nmk], w=[nmk])
                    C.ts(ex[:], ex[:], nm[:, 1:2], None, ALU.mult, None, r=[exk, nmk], w=[exk])
                    gh_, ghk_ = c16.get()
                    C.cp(gh_[:, 0:8], ex[:], r=[exk], w=[ghk_])
                    C.tt(gh_[:, 8:16], ex[:], gh_[:, 0:8], ALU.subtract, r=[exk, ghk_], w=[ghk_])
                    tpf, tpk = psM.get()
                    tp = tpf[:].bitcast(BF16)
                    C.tr(tp[0:16, 0:128], gh_[:], ident_b[:], r=[ghk_, "ident_b"], w=[tpk])
                    C.cp(gT[:, cs], tp[0:16, 0:128], r=[tpk], w=["gT%d" % tb], eng="act")
            P.wait_keys("act", allk(ak, 16))
            rmsnorm(8, uT, uk, router=router)
            for e_ in range(NEXP):
                for tb in range(NTB):
                    bp, bpk = psM.get()
                    C.mm(bp[:], sel[:, e_ * 128:(e_ + 1) * 128], gT[:, tbs(tb)], True, True, r=["sel", "gT%d" % tb], w=[bpk])
                    C.cp(gbc[:, tbs(tb)], bp[:], r=[bpk], w=["gbc%d" % tb], eng="act")

                def cons_e(mc, tb, pss):
                    (p1, p1k), (p2, p2k) = pss
                    sg_, sgk_ = tmp.get()
                    C.act(sg_[:], p1[:], AF.Silu, r=[p1k], w=[sgk_])
                    C.tt(sg_[:], sg_[:], p2[:], ALU.mult, r=[sgk_, p2k], w=[sgk_])
                    C.tt(A3[:, mc, tbs(tb)], sg_[:], gbc[:, tbs(tb)], ALU.mult, r=[sgk_, "gbc%d" % tb], w=[ak(mc, tb)])
                linear([(ewg[e_], uT, uk, 8), (ewu[e_], uT, uk, 8)], DFE, cons_e)
                linear([(ewd[e_], A3, ak, 11)], D, cons_res)

        rmsnorm(16, uT, uk)
        pT = A3[:, 0:2, :]
        P.dma("pool", pT, pT_d.rearrange("(c p) t -> p c t", p=128), w=[ak(c, tb) for c in range(2) for tb in range(NTB)], slot="pload")

        def cons_p(mc, tb, pss):
            (p1, p1k), (p2, p2k) = pss
            sg_, sgk_ = tmp.get()
            C.act(sg_[:], p1[:], AF.Sigmoid, r=[p1k], w=[sgk_])
            C.tt(sg_[:], sg_[:], p2[:], ALU.mult, r=[sgk_, p2k], w=[sgk_])
            C.tt(hT[:, mc, tbs(tb)], hT[:, mc, tbs(tb)], sg_[:], ALU.add, r=[hk(mc, tb), sgk_], w=[hk(mc, tb)])
        linear([(w_pleg, uT, uk, 8), (w_ple, pT, ak, 2)], D, cons_p)

    outs = []
    if post == "u":
        uT_o = dt_out("uT", [D, TOK], BF16)
        rmsnorm(24, uT, uk)
        uov = uT_o.rearrange("(c p) t -> p c t", p=128)
        for c in range(8):
            P.dma("sp", uov[:, c, :], uT[:, c, :], r=[uk(c, tb) for tb in range(NTB)], slot="uout%d" % c)
        outs += allk(uk, 8)
        if pre:
            hT_o = dt_out("hTo", [D, TOK])
            hov = hT_o.rearrange("(c p) t -> p c t", p=128)
            for c in range(8):
                P.dma("sp", hov[:, c, :], hT[:, c, :], r=[hk(c, tb) for tb in range(NTB)], slot="hout%d" % c)
            outs += allk(hk, 8)
    else:
        oT_o = dt_out("oT", [D, TOK])
        of = arena[:].bitcast(F32).rearrange("p (c t) -> p c t", c=8)
        okf = lambda c, tb: "of%d_%d" % (c, tb)
        P.wait_keys("dve", allk(ak, 16))
        rmsnorm(32, of, okf)
        oov = oT_o.rearrange("(c p) t -> p c t", p=128)
        for c in range(8):
            P.dma("sp", oov[:, c, :], of[:, c, :], r=[okf(c, tb) for tb in range(NTB)], slot="oout%d" % c)
        outs += [okf(c, tb) for c in range(8) for tb in range(NTB)]
    P.wait_keys("sp", outs)
    P.emit()
    return nc


def _fm(a2d):
    return np.ascontiguousarray(a2d.T)


def token_inputs(layer, inp, pre, ffn, post):
    m = {}
    norms = np.zeros((128, 40), np.float32)
    f = lambda v: np.asarray(v, np.float32).reshape(8, 128).T
    norms[:, 0:8] = f(inp["norm_mix"][layer])
    norms[:, 8:16] = f(inp["norm_ffn"][layer])
    norms[:, 16:24] = f(inp["norm_ple"][layer])
    if post == "u":
        norms[:, 24:32] = f(inp["norm_mix"][layer + 1 if pre else layer])
    norms[:, 32:40] = f(inp["norm_final"])
    m["norms"] = norms
    if pre:
        W = inp["w_in"][layer]
        m["w_mg"] = np.ascontiguousarray(W[:, O_MGR:O_MGR + 2 * D])
        m["w_brr"] = inp["w_br_ret"][layer]
        m["w_brg"] = inp["w_br_gdn"][layer]
        m["w_out"] = inp["w_out"][layer]
        m["w_ple"] = inp["w_ple"][layer]
        m["w_pleg"] = inp["w_ple_gate"][layer]
        j = layer // 2
        if ffn == "dense":
            m["wg"] = inp["ffn_w_gate"][j]
            m["wu"] = inp["ffn_w_up"][j]
            m["wd"] = inp["ffn_w_down"][j]
        else:
            m["router"] = inp["router"][j]
            m["ewg"] = inp["exp_w_gate"][j]
            m["ewu"] = inp["exp_w_up"][j]
            m["ewd"] = inp["exp_w_down"][j]
    return m


_PROGS = {}


def _prog(key, builder):
    if key not in _PROGS:
        nc = bass.Bass("TRN2", target_bir_lowering=False)
        builder(nc)
        _PROGS[key] = nc
    return _PROGS[key]


def _run(nc, in_maps):
    res = run_bass_kernel_spmd(nc, in_maps, core_ids=list(range(NCORE)))
    return res.results


def _gather_u(res):
    return [np.ascontiguousarray(np.concatenate([res[b * 4 + s]["uT"] for s in range(4)], axis=1)) for b in range(NB)]


def _shuffle_go(res):
    outs = []
    for c in range(NCORE):
        b, s = c // 4, c % 4
        cols = slice(s * TOK, (s + 1) * TOK)
        ret = [res[b * 4 + hg]["goT"][0:256, cols] for hg in range(4)]
        gdn = [res[b * 4 + hg]["goT"][256:512, cols] for hg in range(4)]
        outs.append(np.ascontiguousarray(np.concatenate(ret + gdn, axis=0)))
    return outs


def kernel(**inp):
    inp = {k: np.asarray(v) for k, v in inp.items()}
    x = inp["x"].astype(np.float32, copy=False)
    p = inp["p"].astype(np.float32, copy=False)
    tok_slices = [(c // 4, slice((c % 4) * TOK, (c % 4 + 1) * TOK)) for c in range(NCORE)]
    t_first = _prog("t_first", lambda nc: build_token(nc, False, None, "u"))
    base0 = token_inputs(0, inp, False, None, "u")
    hT = [_fm(x[b, sl]) for (b, sl) in tok_slices]
    res = _run(t_first, [dict(base0, hT=hT[c]) for c in range(NCORE)])
    for layer in range(2):
        uT_b = _gather_u(res)
        mixer = _prog("mixer", lambda nc: build_mixer(nc))
        mins = []
        for c in range(NCORE):
            mi = mixer_inputs(layer, c % 4, inp["w_in"], inp["conv_w"], inp["a_log"], inp["dt_bias"], inp["ret_gn"], inp["gdn_gn"])
            mi["uT"] = uT_b[c // 4]
            mins.append(mi)
        gres = _run(mixer, mins)
        go = _shuffle_go(gres)
        ffn = "dense" if layer % 2 == 0 else "moe"
        post = "u" if layer == 0 else "final"
        prog = _prog("t_%s_%s" % (ffn, post), lambda nc: build_token(nc, True, ffn, post))
        base = token_inputs(layer, inp, True, ffn, post)
        ins = []
        for c in range(NCORE):
            b, sl = tok_slices[c]
            ins.append(dict(base, hT=hT[c], goT=go[c], pT=_fm(p[layer, b, sl])))
        res = _run(prog, ins)
        if layer == 0:
            hT = [res[c]["hTo"] for c in range(NCORE)]
    out = np.zeros((NB, T, D), np.float32)
    for c in range(NCORE):
        b, sl = tok_slices[c]
        out[b, sl] = res[c]["oT"].T
    return out
```
